# Optimizing a Trainium2 kernel written in Bass

```python
import math
import jax, jax.numpy as jnp
from jax import lax
import numpy as np

D_MODEL = 1024
BATCH = 4
SEQ = 4096
DEPTH = 4

D_MLSTM = D_MODEL
N_MLSTM_HEADS = 4
MLSTM_HEAD_DIM = D_MLSTM // N_MLSTM_HEADS
D_CONV = D_MODEL
N_CONV_GROUPS = 8
D_MIX = D_MLSTM + D_CONV
D_IN_PROJ = 2 * D_MLSTM + 2 * D_CONV
MLSTM_CONV_K = 4
CONFORMER_CONV_K = 31
CHUNK = 128
D_FF = 4 * D_MODEL
N_ADA = 6
EPS = 1e-6

kernel_name = "hybrid_mlstm_conformer_conv_sandwich_adaln"


def rms_norm(x, g):
    xf = x.astype(jnp.float32)
    y = xf * lax.rsqrt(jnp.mean(xf * xf, axis=-1, keepdims=True) + EPS)
    return (y * g.astype(jnp.float32)).astype(x.dtype)


def group_layer_norm(x, n_groups, g, b=None):
    shp = x.shape
    xf = x.astype(jnp.float32).reshape(*shp[:-1], n_groups, shp[-1] // n_groups)
    mu = jnp.mean(xf, axis=-1, keepdims=True)
    xc = xf - mu
    var = jnp.mean(xc * xc, axis=-1, keepdims=True)
    y = (xc * lax.rsqrt(var + EPS)).reshape(shp) * g.astype(jnp.float32)
    if b is not None:
        y = y + b.astype(jnp.float32)
    return y.astype(x.dtype)


def causal_depthwise_conv(x, w, b):
    K, C = w.shape
    y = lax.conv_general_dilated(
        x, w[:, None, :].astype(x.dtype), window_strides=(1,), padding=[(K - 1, 0)],
        dimension_numbers=("NWC", "WIO", "NWC"), feature_group_count=C)
    return y + b.astype(x.dtype)


def mlstm_chunkwise(q, k, v, ig, lf):
    B, T, H, Dh = q.shape
    nc = T // CHUNK

    def to_chunks(a):
        a = a.astype(jnp.float32).reshape(B, nc, CHUNK, H, *a.shape[3:])
        return jnp.moveaxis(a, (1, 3), (0, 2))

    xs = (to_chunks(q), to_chunks(k), to_chunks(v), to_chunks(ig), to_chunks(lf))
    causal = jnp.tril(jnp.ones((CHUNK, CHUNK), dtype=bool))

    def body(carry, inp):
        C, n, m = carry
        qc, kc, vc, igc, lfc = inp
        b = jnp.cumsum(lfc, axis=-1)
        logw = b[..., :, None] - b[..., None, :] + igc[..., None, :]
        logw = jnp.where(causal, logw, -jnp.inf)
        g = b + m[..., None]
        m_t = jnp.maximum(g, jnp.max(logw, axis=-1))
        w = jnp.exp(logw - m_t[..., None])
        inter = jnp.exp(g - m_t)
        s = jnp.einsum("bhtd,bhsd->bhts", qc, kc) * w
        num = jnp.einsum("bhts,bhse->bhte", s, vc) + inter[..., None] * jnp.einsum("bhtd,bhde->bhte", qc, C)
        den = jnp.sum(s, axis=-1) + inter * jnp.einsum("bhtd,bhd->bht", qc, n)
        h = num / jnp.maximum(jnp.abs(den), jnp.exp(-m_t))[..., None]
        b_last = b[..., -1]
        a = b_last[..., None] - b + igc
        m_new = jnp.maximum(b_last + m, jnp.max(a, axis=-1))
        wk = jnp.exp(a - m_new[..., None])
        decay = jnp.exp(b_last + m - m_new)
        C_new = decay[..., None, None] * C + jnp.einsum("bhs,bhsd,bhse->bhde", wk, kc, vc)
        n_new = decay[..., None] * n + jnp.einsum("bhs,bhsd->bhd", wk, kc)
        return (C_new, n_new, m_new), h

    init = (jnp.zeros((B, H, Dh, Dh), jnp.float32), jnp.zeros((B, H, Dh), jnp.float32),
            jnp.zeros((B, H), jnp.float32))
    _, hs = lax.scan(body, init, xs)
    return jnp.moveaxis(hs, (0, 2), (1, 3)).reshape(B, T, H, Dh)


def mixer(h, w_in, w_conv_m, b_conv_m, w_q, w_k, w_v, w_gates, b_gates, g_mh,
          w_dw, b_dw, g_cn, b_cn, w_out):
    B, T, _ = h.shape
    H, Dh = N_MLSTM_HEADS, MLSTM_HEAD_DIM
    u = h @ w_in.astype(h.dtype)
    x_m, z, glu_a, glu_b = jnp.split(u, [D_MLSTM, 2 * D_MLSTM, 2 * D_MLSTM + D_CONV], axis=-1)

    x_c = jax.nn.silu(causal_depthwise_conv(x_m, w_conv_m, b_conv_m))
    xc_h = x_c.reshape(B, T, H, Dh)
    xm_h = x_m.reshape(B, T, H, Dh)
    q = jnp.einsum("bthd,hde->bthe", xc_h, w_q.astype(h.dtype))
    k = jnp.einsum("bthd,hde->bthe", xc_h, w_k.astype(h.dtype))
    v = jnp.einsum("bthd,hde->bthe", xm_h, w_v.astype(h.dtype))
    qkv = jnp.concatenate([q, k, v], axis=-1).reshape(B, T, H, 3 * Dh)
    qkv = jnp.moveaxis(qkv, 2, 3).reshape(B, T, 3 * D_MLSTM)
    gates = (qkv @ w_gates.astype(h.dtype) + b_gates.astype(h.dtype)).astype(jnp.float32)
    ig, fg = gates[..., :H], gates[..., H:]
    lf = jax.nn.log_sigmoid(fg)
    h_m = mlstm_chunkwise(q, k * (Dh ** -0.5), v, ig, lf)
    h_m = group_layer_norm(h_m.reshape(B, T, D_MLSTM), H, g_mh).astype(h.dtype)
    h_m = jax.nn.sigmoid(z) * h_m

    a = glu_a * jax.nn.sigmoid(glu_b)
    a = causal_depthwise_conv(a, w_dw, b_dw)
    a = jax.nn.silu(group_layer_norm(a, N_CONV_GROUPS, g_cn, b_cn))

    return jnp.concatenate([h_m, a], axis=-1) @ w_out.astype(h.dtype)


def setup_inputs(seed: int = 0) -> dict:
    key = jax.random.key(seed)
    ks = jax.random.split(key, 26)
    f32 = jnp.float32

    def nrm(k, shape, fan_in, scale=1.0):
        return jax.random.normal(k, shape, f32) * (scale * fan_in ** -0.5)

    def gain(k, shape):
        return 1.0 + 0.05 * jax.random.normal(k, shape, f32)

    def small(k, shape):
        return 0.02 * jax.random.normal(k, shape, f32)

    H = N_MLSTM_HEADS
    ig_bias = 0.1 * jax.random.normal(ks[24], (DEPTH, H), f32)
    fg_bias = jnp.linspace(3.0, 6.0, H, dtype=f32)[None, :] + 0.1 * jax.random.normal(ks[25], (DEPTH, H), f32)
    return {
        "x": jax.random.normal(ks[0], (BATCH, SEQ, D_MODEL), f32),
        "c": jax.random.normal(ks[1], (BATCH, D_MODEL), f32),
        "w_ada": nrm(ks[2], (DEPTH, D_MODEL, N_ADA * D_MODEL), D_MODEL, 0.5),
        "b_ada": small(ks[3], (DEPTH, N_ADA * D_MODEL)),
        "g_pre_mix": gain(ks[4], (DEPTH, D_MODEL)),
        "g_post_mix": gain(ks[5], (DEPTH, D_MODEL)),
        "g_pre_mlp": gain(ks[6], (DEPTH, D_MODEL)),
        "g_post_mlp": gain(ks[7], (DEPTH, D_MODEL)),
        "w_in": nrm(ks[8], (DEPTH, D_MODEL, D_IN_PROJ), D_MODEL),
        "w_conv_m": nrm(ks[9], (DEPTH, MLSTM_CONV_K, D_MLSTM), MLSTM_CONV_K),
        "b_conv_m": small(ks[10], (DEPTH, D_MLSTM)),
        "w_q": nrm(ks[11], (DEPTH, H, MLSTM_HEAD_DIM, MLSTM_HEAD_DIM), MLSTM_HEAD_DIM),
        "w_k": nrm(ks[12], (DEPTH, H, MLSTM_HEAD_DIM, MLSTM_HEAD_DIM), MLSTM_HEAD_DIM),
        "w_v": nrm(ks[13], (DEPTH, H, MLSTM_HEAD_DIM, MLSTM_HEAD_DIM), MLSTM_HEAD_DIM),
        "w_gates": nrm(ks[14], (DEPTH, 3 * D_MLSTM, 2 * H), 3 * D_MLSTM),
        "b_gates": jnp.concatenate([ig_bias, fg_bias], axis=-1),
        "g_mh": gain(ks[15], (DEPTH, D_MLSTM)),
        "w_dw": nrm(ks[16], (DEPTH, CONFORMER_CONV_K, D_CONV), CONFORMER_CONV_K),
        "b_dw": small(ks[17], (DEPTH, D_CONV)),
        "g_cn": gain(ks[18], (DEPTH, D_CONV)),
        "b_cn": small(ks[19], (DEPTH, D_CONV)),
        "w_out": nrm(ks[20], (DEPTH, D_MIX, D_MODEL), D_MIX),
        "w_ff1": nrm(ks[21], (DEPTH, D_MODEL, D_FF), D_MODEL),
        "w_ff2": nrm(ks[22], (DEPTH, D_FF, D_MODEL), D_FF),
    }


def reference(x, c, w_ada, b_ada, g_pre_mix, g_post_mix, g_pre_mlp, g_post_mlp, w_in,
              w_conv_m, b_conv_m, w_q, w_k, w_v, w_gates, b_gates, g_mh, w_dw, b_dw,
              g_cn, b_cn, w_out, w_ff1, w_ff2):
    c_act = jax.nn.silu(c.astype(jnp.float32))
    for l in range(DEPTH):
        mod = (c_act @ w_ada[l].astype(jnp.float32) + b_ada[l].astype(jnp.float32)).astype(x.dtype)
        sh_mix, sc_mix, gt_mix, sh_mlp, sc_mlp, gt_mlp = [m[:, None, :] for m in jnp.split(mod, N_ADA, axis=-1)]

        h = rms_norm(x, g_pre_mix[l]) * (1 + sc_mix) + sh_mix
        y = mixer(h, w_in[l], w_conv_m[l], b_conv_m[l], w_q[l], w_k[l], w_v[l], w_gates[l], b_gates[l],
                  g_mh[l], w_dw[l], b_dw[l], g_cn[l], b_cn[l], w_out[l])
        x = x + gt_mix * rms_norm(y, g_post_mix[l])

        h = rms_norm(x, g_pre_mlp[l]) * (1 + sc_mlp) + sh_mlp
        f = jnp.square(jax.nn.relu(h @ w_ff1[l].astype(h.dtype))) @ w_ff2[l].astype(h.dtype)
        x = x + gt_mlp * rms_norm(f, g_post_mlp[l])
    return x
```

```python
import numpy as np
import concourse.bass as bass
import concourse.mybir as mybir
from concourse.bass_utils import run_bass_kernel_spmd

F32 = mybir.dt.float32
BF = mybir.dt.bfloat16
AF = mybir.ActivationFunctionType
ALU = mybir.AluOpType
AX = mybir.AxisListType

D = 1024
TOK = 2048
TT = 512
NT = TOK // TT
DEPTH = 4
EPS = 1e-6
SW = 2336
OFF_N, OFF_AH, OFF_XH, OFF_MP = 2048, 2056, 2296, 2320
NV = 15
NSLOT = 4
SAME_ENGINE_SYNC = True
EPOCH = 16000


class Buf:
    __slots__ = ("name", "w", "rs", "excl")

    def __init__(self, name, excl=False):
        self.name = name
        self.w = None
        self.rs = []
        self.excl = excl


class V:
    __slots__ = ("ap", "buf")

    def __init__(self, ap, buf):
        self.ap = ap
        self.buf = buf

    def s(self, *idx):
        return V(self.ap[idx], self.buf)


class Op:
    __slots__ = ("eng", "fn", "deps", "seq", "sig", "sigidx", "chan", "chanval")

    def __init__(self, eng, fn):
        self.eng = eng
        self.fn = fn
        self.deps = []
        self.sig = False
        self.sigidx = 0
        self.chan = None
        self.chanval = 0


class Chan:
    def __init__(self, sem):
        self.sem = sem
        self.count = 0


ENGS = ("pe", "act", "dve", "pool", "sp")


class Sched:
    def __init__(self):
        self.ops = {e: [] for e in ENGS}
        self.known = {e: {f: -1 for f in ENGS} for e in ENGS}
        self.knownchan = {e: {} for e in ENGS}

    def add(self, eng, fn, reads, writes, chan=None):
        op = Op(eng, fn)
        op.seq = len(self.ops[eng])
        deps = []
        ex = [v for v in reads if (v.buf if isinstance(v, V) else v).excl]
        if ex:
            reads = [v for v in reads if not (v.buf if isinstance(v, V) else v).excl]
            writes = list(writes) + ex
        for v in reads:
            b = v.buf if isinstance(v, V) else v
            if b.w is not None:
                deps.append(b.w)
        for v in writes:
            b = v.buf if isinstance(v, V) else v
            if b.w is not None:
                deps.append(b.w)
            deps.extend(b.rs)
        for p in deps:
            if p.chan is not None:
                val = p.chan.count
                if self.knownchan[eng].get(id(p.chan), 0) >= val:
                    continue
                self.knownchan[eng][id(p.chan)] = val
                op.deps.append(("chan", p.chan, val))
                continue
            if p.eng == eng:
                if eng in ("pe", "sp") or not SAME_ENGINE_SYNC:
                    continue
            if self.known[eng][p.eng] >= p.seq:
                continue
            self.known[eng][p.eng] = p.seq
            p.sig = True
            op.deps.append(("op", p))
        if chan is not None:
            chan.count += 1
            op.chan = chan
            op.chanval = chan.count
        for v in reads:
            b = v.buf if isinstance(v, V) else v
            b.rs.append(op)
        for v in writes:
            b = v.buf if isinstance(v, V) else v
            b.w = op
            b.rs = []
        self.ops[eng].append(op)
        return op

    def emit(self, nc, block, sems):
        for e in ENGS:
            n = 0
            for op in self.ops[e]:
                if op.sig:
                    n += 1
                    op.sigidx = n
        handles = {"pe": "tensor", "act": "scalar", "dve": "vector", "pool": "gpsimd", "sp": "sync"}

        def run(ename):
            def body(eng):
                for op in self.ops[ename]:
                    for d in op.deps:
                        if d[0] == "chan":
                            eng.wait_ge(d[1].sem, 16 * d[2])
                        else:
                            p = d[1]
                            k = p.sigidx - 1
                            eng.wait_ge(sems[p.eng][k // EPOCH], k % EPOCH + 1)
                    ins = op.fn(eng)
                    if op.chan is not None:
                        ins.then_inc(op.chan.sem, 16)
                    elif op.sig:
                        k = op.sigidx - 1
                        ins.then_inc(sems[ename][k // EPOCH], 1)
            return body

        for e in ENGS:
            getattr(block, handles[e])(run(e))


def build(NS, fused):
    nc = bass.Bass("TRN2", target_bir_lowering=False)
    S = Sched()
    ctx = []

    def dram(name, shape, kind, dt=F32):
        if kind is None:
            return nc.dram_tensor(name, shape, dt).ap()
        return nc.dram_tensor(name, shape, dt, kind=kind).ap()

    xT_d = dram("xT", [128, 8, TOK], "ExternalInput")
    cT_d = dram("cT", [128, 8], "ExternalInput")
    flags_d = dram("flags", [128, 8], "ExternalInput")
    consts_d = dram("consts", [128, 1280], "ExternalInput")
    w_in_d = dram("w_in", [NS, D, 4 * D], "ExternalInput")
    w_out_d = dram("w_out", [NS, 2 * D, D], "ExternalInput")
    w_ff1_d = dram("w_ff1", [NS, D, 4 * D], "ExternalInput")
    w_ff2_d = dram("w_ff2", [NS, 4 * D, D], "ExternalInput")
    w_ada_d = dram("w_ada", [NS, D, 6 * D], "ExternalInput")
    wqkv_d = dram("wqkv", [NS, 3, 4, 256, 256], "ExternalInput")
    wqkvT_d = dram("wqkvT", [NS, 3, 4, 256, 256], "ExternalInput")
    wg_d = dram("wg", [NS, 128, 192], "ExternalInput")
    vecs_d = dram("vecs", [NS, 128, NV * 8], "ExternalInput")
    convw_d = dram("convw", [NS, 128, 8 * 35], "ExternalInput")
    bg_d = dram("bg", [NS, 4, 2], "ExternalInput")
    yT_d = dram("yT", [128, 8, TOK], "ExternalOutput")
    DBGN = ["h", "xm", "zs", "ag", "aout", "xc", "hm", "hmix", "y", "xmix", "h2", "f"] if DEBUG else []
    dbg_d = {n: dram("dbg_" + n, [128, 8, TOK], "ExternalOutput") for n in DBGN}
    DBGS = ["ig", "lf", "bcs", "es", "mpr"] if DEBUG else []
    for n in DBGS:
        dbg_d[n] = dram("dbg_" + n, [4, TOK], "ExternalOutput")
    if DEBUG:
        dbg_d["ecol"] = dram("dbg_ecol", [128, 16 * NT], "ExternalOutput")
        dbg_d["intb"] = dram("dbg_intb", [128, 16 * NT], "ExternalOutput")
        dbg_d["small"] = dram("dbg_small", [4, 32 * NT], "ExternalOutput")
        dbg_d["qk"] = dram("dbg_qk", [128, 8, TOK], "ExternalOutput")
        dbg_d["flb"] = dram("dbg_flb", [128, TOK], "ExternalOutput")
        dbg_d["pfc"] = dram("dbg_pfc", [128, TOK], "ExternalOutput")
    if not fused:
        st_in_d = dram("st_in", [128, SW], "ExternalInput")
        st_out_d = dram("st_out", [128, SW], "ExternalOutput")
    else:
        send_d = [dram(f"cc_send{s}", [128, SW], None) for s in range(NS - 1)]
        recv_d = [dram(f"cc_recv{s}", [256, SW], None) for s in range(NS - 1)]

    import contextlib
    stack = contextlib.ExitStack()

    def sb(name, shape, dt):
        return stack.enter_context(nc.sbuf_tensor("sb_" + name, shape, dt))

    def ps(name, shape, dt=F32):
        return stack.enter_context(nc.psum_tensor(name, shape, dt))

    def sem(name):
        return stack.enter_context(nc.semaphore(name))

    with stack:
        X = sb("X", [128, 8, TOK], F32)
        Xb = [[Buf(f"x{c}_{t}") for t in range(NT)] for c in range(8)]
        SLOTS = [sb(f"wslot{i}", [128, 8, 512], BF) for i in range(NSLOT)]
        SLOTB = [Buf(f"slot{i}") for i in range(NSLOT)]
        slotchan = [Chan(sem(f"slotsem{i}")) for i in range(NSLOT)]

        def cbuf(name, cols=512, dt=BF, parts=128):
            t = sb(name, [parts, cols], dt)
            return V(t[:, :], Buf(name))

        Hb = [cbuf(f"h{c}") for c in range(8)]
        SQ = [cbuf(f"sq{c}") for c in range(8)]
        XM = [cbuf(f"xm{c}", 515) for c in range(8)]
        AG = [cbuf(f"ag{c}", 542) for c in range(8)]
        ZS = [cbuf(f"zs{c}") for c in range(8)]
        XC = [cbuf(f"xc{c}") for c in range(8)]
        PH = [cbuf(f"ph{c}") for c in range(8)]
        FB = [cbuf(f"fb{c}", 512, F32) for c in range(8)]
        R1 = cbuf("r1", 512, F32)
        T1 = cbuf("t1", 512, F32)
        T2 = cbuf("t2", 512, F32)
        M1 = cbuf("m1", 512, F32)
        FLB = cbuf("flb", 512, F32)
        CONST = cbuf("const", 1280, F32)
        IDENT = CONST.s(slice(None), slice(0, 128))
        MASK = CONST.s(slice(None), slice(128, 256))
        IDB = cbuf("identb", 128, BF)
        ONESB = cbuf("onesb", 128, BF)
        FLG = cbuf("flags", 8, F32)
        CCOL = cbuf("ccol", 8, F32)
        CACT = cbuf("cact", 8, BF)
        VEC = cbuf("vecs", NV * 8, F32)
        CW = cbuf("convw", 8 * 35, F32)
        BG = cbuf("bg", 2, F32, 4)
        WG = cbuf("wg", 192, BF)
        MOD = cbuf("mod", 48, F32)
        ADA = cbuf("ada", 48, F32)
        WQK = cbuf("wqkp", 64, BF)
        WV = cbuf("wvp", 64, BF)
        CM = cbuf("cm", SW, F32)
        CHAT = [cbuf(f"chat{d}", 256, BF) for d in range(2)]
        NHAT = [cbuf(f"nhat{d}", 128, BF) for d in range(2)]
        GA = cbuf("ga", 512, F32, 4)
        GBf = cbuf("gb", 512, F32, 4)
        GC = cbuf("gc", 512, F32, 4)
        SMALL = cbuf("small", 32, F32, 4)
        ECOL = cbuf("ecol", 16, F32)
        ECOLB = cbuf("ecolb", 16, BF)
        INTB = cbuf("intb", 16, F32)
        SWT = [cbuf(f"swt{i}", 128, BF) for i in range(2)]
        DD = cbuf("dd", 128, F32)
        EBT = [cbuf(f"ebt{i}", 128, BF) for i in range(2)]
        NDIAG = 8
        DIAG = [cbuf(f"diag{i}", 128, BF) for i in range(NDIAG)]
        PB = []
        for i in range(8):
            t = ps(f"pb{i}", [128, 512])
            PB.append(V(t[:, :], Buf(f"pb{i}", excl=True)))
        print("sbuf bytes remaining:", nc.sbuf_bytes_remaining)

        esems = {e: [sem(f"{e}sem{i}") for i in range(5)] for e in ENGS}
        ldchan = Chan(sem("ldsem"))
        stchan = Chan(sem("stsem"))
        outchan = Chan(sem("outsem"))
        ccchan = Chan(sem("ccsem"))

        def aps(x):
            return x.ap if isinstance(x, V) else x

        def rd(*xs):
            return [x for x in xs if isinstance(x, V)]

        def mm(out, lhsT, rhs, start, stop):
            S.add("pe", lambda e: e.matmul(out.ap, lhsT.ap, rhs.ap, start=start, stop=stop),
                  rd(lhsT, rhs), [out])

        def act(out, in_, func, bias=0.0, scale=1.0):
            S.add("act", lambda e: e.activation(out.ap, in_.ap, func, bias=aps(bias), scale=aps(scale)),
                  rd(in_, bias, scale), [out])

        def tt(out, a, b, op, eng="dve"):
            S.add(eng, lambda e: e.tensor_tensor(out.ap, a.ap, b.ap, op), rd(a, b), [out])

        def ts(out, a, s1, op0, s2=None, op1=None, eng="dve"):
            if op1 is None:
                S.add(eng, lambda e: e.tensor_scalar(out.ap, a.ap, aps(s1), None, op0), rd(a, s1), [out])
            else:
                S.add(eng, lambda e: e.tensor_scalar(out.ap, a.ap, aps(s1), aps(s2), op0, op1),
                      rd(a, s1, s2), [out])

        def stt(out, a, sc, b, op0, op1):
            S.add("dve", lambda e: e.scalar_tensor_tensor(out.ap, a.ap, aps(sc), b.ap, op0, op1),
                  rd(a, sc, b), [out])

        def recip(out, a):
            S.add("dve", lambda e: e.reciprocal(out.ap, a.ap), rd(a), [out])

        def cp(out, a, eng="dve"):
            S.add(eng, lambda e: e.tensor_copy(out.ap, a.ap), rd(a), [out])

        def memset(out, val, eng="dve"):
            S.add(eng, lambda e: e.memset(out.ap, val), [], [out])

        def dma(eng, out_ap, in_ap, reads, writes, chan):
            S.add(eng, lambda e: e.dma_start(out=out_ap, in_=in_ap), reads, writes, chan)

        dbgchan = Chan(sem("dbgsem"))
        dbgbuf = Buf("dbg")

        def dumps(name, t, v, w=TT):
            if DEBUG:
                dma("pool", dbg_d[name][:, t * w:(t + 1) * w], v.ap, [v], [dbgbuf], dbgchan)

        def dump(name, t, vs):
            if not DEBUG:
                return
            for c, v in enumerate(vs):
                dma("pool", dbg_d[name][:, c, t * TT:(t + 1) * TT], v.ap, [v], [dbgbuf], dbgchan)

        rot = [0]

        def bank():
            b = PB[rot[0] % 4]
            rot[0] += 1
            return b

        def col(v, c, w=1):
            return v.s(slice(None), slice(c, c + w))

        pieces = []

        def kn(w_d, s, k0, n0, n=512):
            return w_d[s, k0:k0 + 1024, n0:n0 + n].rearrange("(kc p) n -> p kc n", p=128)

        for s in range(NS):
            for i in range(12):
                pieces.append([((0, 512), kn(w_ada_d, s, 0, 512 * i))])
            pieces.append([((0, 256), wqkvT_d[s, 0].rearrange("h (ec p) d -> p (h ec) d", p=128)),
                           ((256, 512), wqkvT_d[s, 1].rearrange("h (ec p) d -> p (h ec) d", p=128))])
            pieces.append([((0, 256), wqkvT_d[s, 2].rearrange("h (ec p) d -> p (h ec) d", p=128))])
            for t in range(NT):
                for pi in (0, 1, 2, 3, 6, 7, 4, 5):
                    pieces.append([((0, 512), kn(w_in_d, s, 0, 512 * pi))])
                pieces.append([((0, 256), wqkv_d[s, 0].rearrange("h (dc p) e -> p (h dc) e", p=128)),
                               ((256, 512), wqkv_d[s, 1].rearrange("h (dc p) e -> p (h dc) e", p=128))])
                pieces.append([((0, 256), wqkv_d[s, 2].rearrange("h (dc p) e -> p (h dc) e", p=128))])
                for p in range(2):
                    for kg in range(2):
                        pieces.append([((0, 512), kn(w_out_d, s, 1024 * kg, 512 * p))])
                for hh in range(2):
                    for pi in range(4 * hh, 4 * hh + 4):
                        pieces.append([((0, 512), kn(w_ff1_d, s, 0, 512 * pi))])
                    for p in range(2):
                        for g in range(2):
                            pieces.append([((0, 512), kn(w_ff2_d, s, 1024 * (2 * hh + g), 512 * p))])
        pstate = {"issued": 0, "next": 0}

        def issue_piece():
            i = pstate["issued"]
            if i >= len(pieces):
                return
            sl = i % NSLOT
            for (a, b), src in pieces[i]:
                dma("pool", SLOTS[sl][:, :, a:b], src, [], [SLOTB[sl]], slotchan[sl])
            pstate["issued"] += 1

        def next_piece():
            k = pstate["next"]
            if k == 0:
                for _ in range(NSLOT - 1):
                    issue_piece()
            else:
                issue_piece()
            pstate["next"] += 1
            sl = k % NSLOT
            t = SLOTS[sl]
            b = SLOTB[sl]
            return lambda kc, a, w: V(t[:, kc, a:a + w], b)

        dma("sp", CONST.ap, consts_d, [], [CONST], ldchan)
        dma("sp", FLG.ap, flags_d, [], [FLG], ldchan)
        dma("sp", CCOL.ap, cT_d, [], [CCOL], ldchan)
        for c in range(8):
            dma("sp", X[:, c, :], xT_d[:, c, :], [], [Xb[c][t] for t in range(NT)], ldchan)
        cp(IDB, IDENT)
        memset(ONESB, 1.0)
        act(CACT, CCOL, AF.Silu)
        SEL = [V(CONST.ap[0:4, 256 + 128 * h:256 + 128 * (h + 1)], CONST.buf) for h in range(4)]
        RESET = V(CONST.ap[0:4, 768:1280], CONST.buf)
        ISB = col(FLG, 0)
        AMX = V(SMALL.ap[:, 0:4], SMALL.buf)
        MC = V(SMALL.ap[:, 4:8], SMALL.buf)
        MPV = V(SMALL.ap[:, 8:13], SMALL.buf)
        INTER = V(SMALL.ap[:, 16:20], SMALL.buf)
        DI = V(SMALL.ap[:, 20:24], SMALL.buf)

        def xv(c, t):
            return V(X[:, c, t * TT:(t + 1) * TT], Xb[c][t])

        def vcol(v, c):
            return col(VEC, v * 8 + c)

        def rms_stats(srcs, scale):
            pb = bank()
            for i, q in enumerate(srcs):
                mm(pb, ONESB, q, i == 0, i == len(srcs) - 1)
            ts(T2, pb, scale, ALU.mult, EPS, ALU.add)
            act(T2, T2, AF.Sqrt)
            recip(R1, T2)

        def prenorm(t, gc, shc):
            for c in range(8):
                act(SQ[c], xv(c, t), AF.Square)
            rms_stats(SQ, 1.0 / D)
            for c in range(8):
                tmp = T1 if c % 2 == 0 else M1
                tt(tmp, xv(c, t), R1, ALU.mult)
                act(Hb[c], tmp, AF.Identity, bias=col(ADA, shc + c), scale=col(ADA, gc + c))

        def postnorm_res(t, gtc):
            rms_stats(SQ, 1.0 / D)
            for c in range(8):
                tmp = T1 if c % 2 == 0 else M1
                tt(tmp, FB[c], R1, ALU.mult)
                stt(xv(c, t), tmp, col(ADA, gtc + c), xv(c, t), ALU.mult, ALU.add)

        dctr = [0]

        def diag(c, tap):
            dgm = DIAG[dctr[0] % NDIAG]
            dctr[0] += 1
            ts(dgm, IDB, col(CW, c * 35 + tap), ALU.mult, 1.0, ALU.mult, eng="pool")
            return dgm

        def group_ln_stats(hb_list, sq_list, scale):
            p1 = bank()
            for i, q in enumerate(hb_list):
                mm(p1, ONESB, q, i == 0, i == len(hb_list) - 1)
            p2 = bank()
            for i, q in enumerate(sq_list):
                mm(p2, ONESB, q, i == 0, i == len(sq_list) - 1)
            ts(M1, p1, scale, ALU.mult)
            tt(T2, M1, M1, ALU.mult)
            stt(T2, p2, scale, T2, ALU.mult, ALU.subtract)
            ts(T2, T2, EPS, ALU.add)
            act(T2, T2, AF.Sqrt)
            recip(R1, T2)

        for s in range(NS):
            VALID = col(FLG, 1 + s)
            dma("sp", VEC.ap, vecs_d[s], [], [VEC], ldchan)
            dma("sp", CW.ap, convw_d[s], [], [CW], ldchan)
            dma("sp", BG.ap, bg_d[s], [], [BG], ldchan)
            dma("pool", WG.ap, wg_d[s], [], [WG], ldchan)
            pmod = bank()
            for i in range(12):
                W = next_piece()
                for j in range(4):
                    for kc in range(8):
                        mm(col(pmod, i * 4 + j), W(kc, j * 128, 128), col(CACT, kc), kc == 0, kc == 7)
            tt(MOD, V(pmod.ap[:, 0:48], pmod.buf), V(VEC.ap[:, 0:48], VEC.buf), ALU.add)
            def sl8(v, a):
                return V(v.ap[:, a:a + 8], v.buf)
            stt(sl8(ADA, 0), sl8(MOD, 8), 1.0, sl8(VEC, 6 * 8), ALU.add, ALU.mult)
            cp(sl8(ADA, 8), sl8(MOD, 0))
            tt(sl8(ADA, 16), sl8(MOD, 16), sl8(VEC, 7 * 8), ALU.mult)
            ts(sl8(ADA, 16), sl8(ADA, 16), VALID, ALU.mult)
            stt(sl8(ADA, 24), sl8(MOD, 32), 1.0, sl8(VEC, 8 * 8), ALU.add, ALU.mult)
            cp(sl8(ADA, 32), sl8(MOD, 24))
            tt(sl8(ADA, 40), sl8(MOD, 40), sl8(VEC, 9 * 8), ALU.mult)
            ts(sl8(ADA, 40), sl8(ADA, 40), VALID, ALU.mult)
            pw = bank()
            Wa = next_piece()
            Wb = next_piece()
            for j in range(3):
                for h in range(4):
                    for dc in range(2):
                        g = j * 8 + h * 2 + dc
                        for ec in range(2):
                            src = Wa if j < 2 else Wb
                            base = 256 * j if j < 2 else 0
                            mm(V(pw.ap[:, g * 8:g * 8 + 8], pw.buf),
                               src(h * 2 + ec, base + dc * 128, 128),
                               V(WG.ap[:, (j * 8 + h * 2 + ec) * 8:(j * 8 + h * 2 + ec) * 8 + 8], WG.buf),
                               ec == 0, ec == 1)
            cp(V(T1.ap[:, 0:192], T1.buf), V(pw.ap[:, 0:192], pw.buf))
            tt(WQK, V(T1.ap[:, 0:64], T1.buf), V(T1.ap[:, 64:128], T1.buf), ALU.add)
            cp(WV, V(T1.ap[:, 128:192], T1.buf))
            if not fused:
                dma("sp", CM.ap, st_in_d, [], [CM], ldchan)
            elif s == 0:
                memset(CM, 0.0)
            else:
                dma("sp", CM.ap, recv_d[s - 1][0:128, :], [ccbuf], [CM], ldchan)
            ts(CM, CM, ISB, ALU.mult)
            for c in range(8):
                act(V(AG[c].ap[:, 0:30], AG[c].buf), V(CM.ap[:, OFF_AH + 30 * c:OFF_AH + 30 * c + 30], CM.buf),
                    AF.Copy)
                act(V(XM[c].ap[:, 0:3], XM[c].buf), V(CM.ap[:, OFF_XH + 3 * c:OFF_XH + 3 * c + 3], CM.buf),
                    AF.Copy)
            cp(V(MPV.ap[:, 0:1], MPV.buf), V(CM.ap[0:4, OFF_MP:OFF_MP + 1], CM.buf))

            for t in range(NT):
                prenorm(t, 0, 8)
                dump("h", t, Hb)
                for pi in (0, 1, 2, 3, 6, 7, 4, 5):
                    W = next_piece()
                    for j in range(4):
                        oc = 4 * pi + j
                        pb = bank()
                        for kc in range(8):
                            mm(pb, W(kc, j * 128, 128), Hb[kc], kc == 0, kc == 7)
                        if oc < 8:
                            act(V(XM[oc].ap[:, 3:515], XM[oc].buf), pb, AF.Copy)
                        elif oc < 16:
                            act(ZS[oc - 8], pb, AF.Sigmoid)
                        elif oc >= 24:
                            act(XC[oc - 24], pb, AF.Sigmoid)
                        else:
                            c = oc - 16
                            tt(V(AG[c].ap[:, 30:542], AG[c].buf), pb, XC[c], ALU.mult)
                dump("xm", t, [V(XM[c].ap[:, 3:515], XM[c].buf) for c in range(8)])
                dump("zs", t, ZS)
                dump("ag", t, [V(AG[c].ap[:, 30:542], AG[c].buf) for c in range(8)])
                for c in range(8):
                    pb = bank()
                    for j in range(31):
                        dgm = diag(c, 4 + j)
                        mm(pb, dgm, V(AG[c].ap[:, j:j + 512], AG[c].buf), j == 0, j == 30)
                    act(T1, pb, AF.Identity, bias=vcol(12, c))
                    cp(V(AG[c].ap[:, 0:30], AG[c].buf), V(AG[c].ap[:, 512:542], AG[c].buf), eng="pool")
                    act(SQ[0], T1, AF.Copy)
                    act(SQ[1], T1, AF.Square)
                    group_ln_stats([SQ[0]], [SQ[1]], 1.0 / 128)
                    tt(T1, T1, M1, ALU.subtract)
                    tt(T1, T1, R1, ALU.mult)
                    act(V(AG[c].ap[:, 30:542], AG[c].buf), T1, AF.Silu, bias=vcol(14, c), scale=vcol(13, c))
                for c in range(8):
                    pb = bank()
                    for j in range(4):
                        dgm = diag(c, j)
                        mm(pb, dgm, V(XM[c].ap[:, j:j + 512], XM[c].buf), j == 0, j == 3)
                    act(XC[c], pb, AF.Silu, bias=vcol(10, c))
                dump("aout", t, [V(AG[c].ap[:, 30:542], AG[c].buf) for c in range(8)])
                dump("xc", t, XC)
                pig = bank()
                pfg = bank()
                for (pg, o) in ((pig, 0), (pfg, 4)):
                    for c in range(8):
                        mm(V(pg.ap[0:4, :], pg.buf), V(WQK.ap[:, c * 8 + o:c * 8 + o + 4], WQK.buf), XC[c],
                           c == 0, False)
                    for c in range(8):
                        mm(V(pg.ap[0:4, :], pg.buf), V(WV.ap[:, c * 8 + o:c * 8 + o + 4], WV.buf),
                           V(XM[c].ap[:, 3:515], XM[c].buf), False, c == 7)
                act(GA, V(pig.ap[0:4, :], pig.buf), AF.Identity, bias=col(BG, 0))
                act(GBf, V(pfg.ap[0:4, :], pfg.buf), AF.Identity, bias=col(BG, 1))
                dumps("ig", t, GA)
                act(GC, GBf, AF.Abs)
                act(GC, GC, AF.Exp, scale=-1.0)
                act(GC, GC, AF.Ln, bias=1.0)
                ts(GBf, GBf, 0.0, ALU.min)
                tt(GBf, GBf, GC, ALU.subtract)
                S.add("dve", lambda e: e.tensor_tensor_scan(GC.ap, RESET.ap, GBf.ap, 0.0, ALU.mult, ALU.add),
                      [RESET, GBf], [GC])
                dumps("lf", t, GBf)
                dumps("bcs", t, GC)
                tt(GA, GA, GC, ALU.subtract)
                S.add("dve", lambda e: e.tensor_reduce(AMX.ap, GA.ap.rearrange("p (c t) -> p c t", t=128),
                                                       AX.X, ALU.max), [GA], [AMX])
                for cc in range(4):
                    tt(col(MC, cc), col(MPV, cc), col(AMX, cc), ALU.max)
                    tt(col(MPV, cc + 1), col(MC, cc), V(GC.ap[:, 128 * cc + 127:128 * cc + 128], GC.buf), ALU.add)
                tt(DI, V(MPV.ap[:, 0:4], MPV.buf), MC, ALU.subtract)
                act(INTER, DI, AF.Exp)
                cp(col(MPV, 0), col(MPV, 4))
                mcb = MC.ap.unsqueeze(2).to_broadcast([4, 4, 128])
                S.add("dve", lambda e: e.tensor_tensor(GA.ap.rearrange("p (c t) -> p c t", t=128),
                                                       GA.ap.rearrange("p (c t) -> p c t", t=128), mcb,
                                                       ALU.subtract), [GA, MC], [GA])
                act(GA, GA, AF.Exp)
                S.add("dve", lambda e: e.tensor_tensor(GC.ap.rearrange("p (c t) -> p c t", t=128),
                                                       GC.ap.rearrange("p (c t) -> p c t", t=128), mcb,
                                                       ALU.add), [GC, MC], [GC])
                dumps("es", t, GA)
                dumps("mpr", t, GC)
                dumps("small", t, SMALL, 32)
                pe_ = bank()
                for cc in range(4):
                    mm(V(pe_.ap[:, cc * 4:cc * 4 + 4], pe_.buf), V(GA.ap[:, cc * 128:(cc + 1) * 128], GA.buf),
                       V(IDENT.ap[0:4, 0:4], IDENT.buf), True, True)
                cp(ECOL, V(pe_.ap[:, 0:16], pe_.buf))
                cp(ECOLB, ECOL)
                pi_ = bank()
                for h in range(4):
                    mm(V(pi_.ap[:, h * 4:h * 4 + 4], pi_.buf), SEL[h], INTER, True, True)
                cp(INTB, V(pi_.ap[:, 0:16], pi_.buf))

                dumps("ecol", t, ECOL, 16)
                dumps("intb", t, INTB, 16)
                WQKp = next_piece()
                WVp = next_piece()
                for h in range(4):
                    qT = [PH[0], PH[1]]
                    kT = [PH[2], PH[3]]
                    ktok = [V(PH[4 + tc // 2].ap[:, 256 * (tc % 2):256 * (tc % 2) + 256], PH[4 + tc // 2].buf)
                            for tc in range(4)]
                    vtok = [V(PH[6 + tc // 2].ap[:, 256 * (tc % 2):256 * (tc % 2) + 256], PH[6 + tc // 2].buf)
                            for tc in range(4)]
                    for ec in range(2):
                        pb = bank()
                        for dc in range(2):
                            mm(pb, WQKp(h * 2 + dc, ec * 128, 128), XC[2 * h + dc], dc == 0, dc == 1)
                        act(qT[ec], pb, AF.Copy)
                        pb = bank()
                        for dc in range(2):
                            mm(pb, WQKp(h * 2 + dc, 256 + ec * 128, 128), XC[2 * h + dc], dc == 0, dc == 1)
                        act(kT[ec], pb, AF.Copy)
                    for half in range(2):
                        pb = bank()
                        for q in range(2):
                            tc = half * 2 + q
                            for dc in range(2):
                                mm(V(pb.ap[:, 256 * q:256 * q + 256], pb.buf),
                                   V(XC[2 * h + dc].ap[:, tc * 128:(tc + 1) * 128], XC[2 * h + dc].buf),
                                   WQKp(h * 2 + dc, 256, 256), dc == 0, dc == 1)
                        act(PH[4 + half], pb, AF.Identity, scale=1.0 / 16)
                        pb = bank()
                        for q in range(2):
                            tc = half * 2 + q
                            for dc in range(2):
                                mm(V(pb.ap[:, 256 * q:256 * q + 256], pb.buf),
                                   V(XM[2 * h + dc].ap[:, 3 + tc * 128:3 + (tc + 1) * 128], XM[2 * h + dc].buf),
                                   WVp(h * 2 + dc, 0, 256), dc == 0, dc == 1)
                        for q in range(2):
                            tc = half * 2 + q
                            ts(vtok[tc], V(pb.ap[:, 256 * q:256 * q + 256], pb.buf), col(ECOL, tc * 4 + h), ALU.mult)
                    if DEBUG:
                        for i_ in range(4):
                            dma("pool", dbg_d["qk"][:, (h % 2) * 4 + i_, t * TT:(t + 1) * TT], PH[i_].ap, [PH[i_]], [dbgbuf], dbgchan)
                    pf = bank()
                    mm(pf, SEL[h], GC, True, True)
                    if DEBUG and h == 0:
                        act(T1, pf, AF.Copy)
                        dumps("pfc", t, T1)
                    act(FLB, pf, AF.Exp, scale=-1.0)
                    if DEBUG and h == 0:
                        dumps("flb", t, FLB)
                    for cc in range(4):
                        ic = col(INTB, h * 4 + cc)
                        ei = cc * 4 + h
                        tsl = slice(cc * 128, (cc + 1) * 128)
                        for dc in range(2):
                            cmv = V(CM.ap[:, (h * 2 + dc) * 256:(h * 2 + dc + 1) * 256], CM.buf)
                            act(CHAT[dc], cmv, AF.Identity, scale=ic)
                            nv_ = V(CM.ap[:, OFF_N + h * 2 + dc:OFF_N + h * 2 + dc + 1].to_broadcast([128, 128]), CM.buf)
                            act(NHAT[dc], nv_, AF.Identity, scale=ic)
                        pS, pN, pD, pU = PB[4], PB[5], PB[6], PB[7]
                        pSv = V(pS.ap[:, 0:128], pS.buf)
                        for ec in range(2):
                            mm(pSv, V(kT[ec].ap[:, tsl], kT[ec].buf), V(qT[ec].ap[:, tsl], qT[ec].buf), ec == 0, ec == 1)
                        sw = SWT[(h * 4 + cc) % 2]
                        stt(sw, pSv, 1.0 / 16, MASK, ALU.mult, ALU.mult)
                        eb = EBT[(h * 4 + cc) % 2]
                        act(eb, ONESB, AF.Identity, scale=col(ECOL, ei))
                        for j in range(2):
                            pNv = V(pN.ap[:, 128 * j:128 * j + 128], pN.buf)
                            mm(pNv, V(vtok[cc].ap[:, 128 * j:128 * j + 128], vtok[cc].buf), sw, True, False)
                            for dc in range(2):
                                mm(pNv, V(CHAT[dc].ap[:, 128 * j:128 * j + 128], CHAT[dc].buf),
                                   V(qT[dc].ap[:, tsl], qT[dc].buf), False, dc == 1)
                        pDv = V(pD.ap[:, 0:128], pD.buf)
                        mm(pDv, eb, sw, True, False)
                        for dc in range(2):
                            mm(pDv, NHAT[dc], V(qT[dc].ap[:, tsl], qT[dc].buf), False, dc == 1)
                        for dc in range(2):
                            mm(V(pD.ap[:, 128 + dc:129 + dc], pD.buf),
                               V(ktok[cc].ap[:, 128 * dc:128 * dc + 128], ktok[cc].buf), col(ECOLB, ei), True, True)
                        for dc in range(2):
                            mm(V(pU.ap[:, 256 * dc:256 * dc + 256], pU.buf),
                               V(ktok[cc].ap[:, 128 * dc:128 * dc + 128], ktok[cc].buf), vtok[cc], True, True)
                        act(DD, pDv, AF.Abs)
                        tt(DD, DD, V(FLB.ap[:, tsl], FLB.buf), ALU.max)
                        recip(DD, DD)
                        for j in range(2):
                            tt(V(FB[2 * h + j].ap[:, tsl], FB[2 * h + j].buf),
                               V(pN.ap[:, 128 * j:128 * j + 128], pN.buf), DD, ALU.mult)
                        for dc in range(2):
                            cmv = V(CM.ap[:, (h * 2 + dc) * 256:(h * 2 + dc + 1) * 256], CM.buf)
                            stt(cmv, cmv, ic, V(pU.ap[:, 256 * dc:256 * dc + 256], pU.buf), ALU.mult, ALU.add)
                        nv2 = V(CM.ap[:, OFF_N + h * 2:OFF_N + h * 2 + 2], CM.buf)
                        stt(nv2, nv2, ic, V(pD.ap[:, 128:130], pD.buf), ALU.mult, ALU.add)
                    dump_hm = [FB[2 * h], FB[2 * h + 1]]
                    if DEBUG:
                        for j in range(2):
                            dma("pool", dbg_d["hm"][:, 2 * h + j, t * TT:(t + 1) * TT], dump_hm[j].ap, [dump_hm[j]], [dbgbuf], dbgchan)
                    for j in range(2):
                        act(SQ[2 + j], FB[2 * h + j], AF.Copy)
                        act(SQ[4 + j], FB[2 * h + j], AF.Square)
                    group_ln_stats([SQ[2], SQ[3]], [SQ[4], SQ[5]], 1.0 / 256)
                    for j in range(2):
                        c = 2 * h + j
                        tt(T1, FB[c], M1, ALU.subtract)
                        tt(T1, T1, R1, ALU.mult)
                        stt(Hb[c], T1, vcol(11, c), ZS[c], ALU.mult, ALU.mult)
                dump("hmix", t, Hb)
                for c in range(8):
                    cp(V(XM[c].ap[:, 0:3], XM[c].buf), V(XM[c].ap[:, 512:515], XM[c].buf), eng="pool")
                if t == NT - 1:
                    for c in range(8):
                        act(V(CM.ap[:, OFF_AH + 30 * c:OFF_AH + 30 * c + 30], CM.buf), V(AG[c].ap[:, 0:30], AG[c].buf),
                            AF.Copy)
                        act(V(CM.ap[:, OFF_XH + 3 * c:OFF_XH + 3 * c + 3], CM.buf), V(XM[c].ap[:, 0:3], XM[c].buf),
                            AF.Copy)
                    cp(V(CM.ap[0:4, OFF_MP:OFF_MP + 1], CM.buf), col(MPV, 0))
                    if not fused:
                        stbuf = Buf("st_out")
                        dma("sp", st_out_d, CM.ap, [CM], [stbuf], outchan)
                    elif s < NS - 1:
                        sbuf_ = Buf(f"send{s}")
                        ccbuf = Buf(f"recv{s}")
                        dma("sp", send_d[s], CM.ap, [CM], [sbuf_], stchan)
                        sd, rv = send_d[s], recv_d[s]
                        S.add("pool", lambda e, sd=sd, rv=rv: e.collective_compute(
                            "AllGather", ALU.bypass, replica_groups=[[0, 1], [2, 3], [4, 5], [6, 7]],
                            ins=[sd], outs=[rv]), [sbuf_], [ccbuf], ccchan)
                for p in range(2):
                    pbs = [bank() for _ in range(4)]
                    for kg in range(2):
                        W = next_piece()
                        for j in range(4):
                            for kc in range(8):
                                kk = kg * 8 + kc
                                src = Hb[kk] if kk < 8 else V(AG[kk - 8].ap[:, 30:542], AG[kk - 8].buf)
                                mm(pbs[j], W(kc, j * 128, 128), src, kk == 0, kk == 15)
                    for j in range(4):
                        oc = 4 * p + j
                        act(FB[oc], pbs[j], AF.Copy)
                        act(SQ[oc], pbs[j], AF.Square)
                dump("y", t, FB)
                postnorm_res(t, 16)
                dump("xmix", t, [xv(c, t) for c in range(8)])
                prenorm(t, 24, 32)
                dump("h2", t, Hb)
                F1 = PH + XC
                for hh in range(2):
                    for q in range(4):
                        W = next_piece()
                        for j in range(4):
                            pb = bank()
                            for kc in range(8):
                                mm(pb, W(kc, j * 128, 128), Hb[kc], kc == 0, kc == 7)
                            tmp = SQ[(q * 4 + j) % 2]
                            act(tmp, pb, AF.Relu)
                            tt(F1[q * 4 + j], tmp, tmp, ALU.mult, eng="pool")
                    for p in range(2):
                        pbs = [bank() for _ in range(4)]
                        for g in range(2):
                            W = next_piece()
                            for j in range(4):
                                for kc in range(8):
                                    mm(pbs[j], W(kc, j * 128, 128), F1[g * 8 + kc], g == 0 and kc == 0,
                                       g == 1 and kc == 7)
                        for j in range(4):
                            oc = 4 * p + j
                            if hh == 0:
                                act(FB[oc], pbs[j], AF.Copy)
                            else:
                                tt(FB[oc], FB[oc], pbs[j], ALU.add)
                                act(SQ[2 + oc] if oc < 6 else Hb[oc - 6], FB[oc], AF.Square)
                dump("f", t, FB)
                sqs = [SQ[2 + oc] if oc < 6 else Hb[oc - 6] for oc in range(8)]
                pb = bank()
                for i, q in enumerate(sqs):
                    mm(pb, ONESB, q, i == 0, i == 7)
                ts(T2, pb, 1.0 / D, ALU.mult, EPS, ALU.add)
                act(T2, T2, AF.Sqrt)
                recip(R1, T2)
                for c in range(8):
                    tmp = T1 if c % 2 == 0 else M1
                    tt(tmp, FB[c], R1, ALU.mult)
                    stt(xv(c, t), tmp, col(ADA, 40 + c), xv(c, t), ALU.mult, ALU.add)

        ybuf = Buf("yT")
        for c in range(8):
            dma("sp", yT_d[:, c, :], X[:, c, :], [Xb[c][t] for t in range(NT)], [ybuf], outchan)
        fin = Buf("fin")
        S.add("sp", lambda e: e.nop(), [ybuf, dbgbuf] + ([stbuf] if not fused else []), [fin])

        with nc.Block() as block:
            S.emit(nc, block, esems)
        print("ops:", {e: len(S.ops[e]) for e in ENGS})
    return nc


def _consts():
    c = np.zeros((128, 1280), np.float32)
    c[:, 0:128] = np.eye(128, dtype=np.float32)
    s = np.arange(128)[:, None]
    t = np.arange(128)[None, :]
    c[:, 128:256] = (s <= t).astype(np.float32)
    for h in range(4):
        c[h, 256 + 128 * h:256 + 128 * (h + 1)] = 1.0
    r = np.ones((512,), np.float32)
    r[0::128] = 0.0
    c[0:4, 768:1280] = r[None, :]
    return c


def _colmajor(v):
    return np.ascontiguousarray(v.reshape(8, 128).T)


def _layer_pack(inp, l):
    f = np.float32
    vec = np.zeros((128, NV * 8), f)
    b_ada = inp["b_ada"][l].reshape(6, 1024)
    for i in range(6):
        vec[:, i * 8:(i + 1) * 8] = _colmajor(b_ada[i])
    names = ["g_pre_mix", "g_post_mix", "g_pre_mlp", "g_post_mlp", "b_conv_m", "g_mh", "b_dw", "g_cn", "b_cn"]
    for i, n in enumerate(names):
        vec[:, (6 + i) * 8:(7 + i) * 8] = _colmajor(inp[n][l])
    cw = np.zeros((128, 8, 35), f)
    cw[:, :, 0:4] = inp["w_conv_m"][l].reshape(4, 8, 128).transpose(2, 1, 0)
    cw[:, :, 4:35] = inp["w_dw"][l].reshape(31, 8, 128).transpose(2, 1, 0)
    wg = inp["w_gates"][l].reshape(3, 256, 4, 8).transpose(0, 2, 1, 3).reshape(24, 128, 8)
    wg = wg.transpose(1, 0, 2).reshape(128, 192)
    bg = np.ascontiguousarray(inp["b_gates"][l].reshape(2, 4).T)
    wqkv = np.stack([inp["w_q"][l], inp["w_k"][l], inp["w_v"][l]])
    wqkvT = np.ascontiguousarray(wqkv.transpose(0, 1, 3, 2))
    return dict(vecs=vec, convw=np.ascontiguousarray(cw.reshape(128, 280)), wg=np.ascontiguousarray(wg), bg=bg,
                wqkv=np.ascontiguousarray(wqkv), wqkvT=wqkvT)


_NC_CACHE = {}


def _get_nc(NS, fused):
    key = (NS, fused)
    if key not in _NC_CACHE:
        _NC_CACHE[key] = build(NS, fused)
    return _NC_CACHE[key]


FUSED = False
DEBUG = False
DBG_OUT = {}
DEBUG_SLOTS = None


def kernel(**inputs):
    inp = {k: np.asarray(v, dtype=np.float32) for k, v in inputs.items()}
    x = inp["x"]
    B = x.shape[0]
    packs = [_layer_pack(inp, l) for l in range(DEPTH)]
    consts = _consts()
    big = ["w_in", "w_out", "w_ff1", "w_ff2", "w_ada"]
    xT = []
    for k in range(8):
        b, half = k // 2, k % 2
        xs = x[b, half * TOK:(half + 1) * TOK, :]
        xT.append(np.ascontiguousarray(xs.reshape(TOK, 8, 128).transpose(2, 1, 0)))
    cT = [_colmajor(inp["c"][k // 2]) for k in range(8)]

    def flags(half, slots):
        fl = np.zeros((128, 8), np.float32)
        fl[:, 0] = float(half)
        for i, s in enumerate(slots):
            l = s - half
            fl[:, 1 + i] = 1.0 if 0 <= l < DEPTH else 0.0
        return fl

    def lay(s, half):
        return min(max(s - half, 0), DEPTH - 1)

    if FUSED:
        NS = DEPTH + 1
        nc = _get_nc(NS, True)
        shared = {}
        for half in range(2):
            ls = [lay(s, half) for s in range(NS)]
            d = {n: np.ascontiguousarray(inp[n][ls]) for n in big}
            for n in ["wqkv", "wqkvT", "wg", "vecs", "convw", "bg"]:
                d[n] = np.stack([packs[l][n] for l in ls])
            shared[half] = d
        in_maps = []
        for k in range(8):
            half = k % 2
            m = dict(shared[half])
            m.update(xT=xT[k], cT=cT[k], flags=flags(half, range(NS)), consts=consts)
            in_maps.append(m)
        res = run_bass_kernel_spmd(nc, in_maps, core_ids=list(range(8)))
        outs = [r["yT"] for r in res.results]
    else:
        nc = _get_nc(1, False)
        cur = xT
        state = [np.zeros((128, SW), np.float32) for _ in range(8)]
        for s in range(DEPTH + 1):
            in_maps = []
            for k in range(8):
                half = k % 2
                l = lay(s, half)
                m = {n: inp[n][l:l + 1] for n in big}
                for n in ["wqkv", "wqkvT", "wg", "vecs", "convw", "bg"]:
                    m[n] = packs[l][n][None]
                st = state[k - 1] if half == 1 else np.zeros((128, SW), np.float32)
                m.update(xT=cur[k], cT=cT[k], flags=flags(half, [s]), consts=consts, st_in=st)
                in_maps.append(m)
            res = run_bass_kernel_spmd(nc, in_maps, core_ids=list(range(8)))
            cur = [np.asarray(r["yT"]) for r in res.results]
            if DEBUG:
                for n in res.results[0]:
                    if n.startswith("dbg_"):
                        DBG_OUT[n] = np.asarray(res.results[0][n])
            state = [np.asarray(r["st_out"]) for r in res.results]
            if DEBUG_SLOTS is not None and s + 1 >= DEBUG_SLOTS:
                break
        outs = cur
    out = np.empty_like(x)
    for k in range(8):
        b, half = k // 2, k % 2
        out[b, half * TOK:(half + 1) * TOK, :] = np.asarray(outs[k]).transpose(2, 1, 0).reshape(TOK, D)
    return out
```
